# Optimizing a Trainium2 kernel written in Bass

```python
import jax, jax.numpy as jnp
from jax import lax
import numpy as np

D_MODEL = 1024
BATCH = 8
SEQ = 4096
DEPTH = 2

GRID_W = 64
CTX_LEN = 256
N_MIXERS = 2
N_HEADS = 8
N_KV_HEADS = 2
GROUP = N_HEADS // N_KV_HEADS
HEAD_DIM = 128
ATTN_DIM = N_HEADS * HEAD_DIM
QKV_DIM = (N_HEADS + 2 * N_KV_HEADS) * HEAD_DIM
WINDOW = 128
BLOCK = 128
AXIS_DIM = HEAD_DIM // 2
ROPE_BASE = 10000.0
LRU_WIDTH = D_MODEL
LRU_BLOCKS = 8
LRU_BLOCK_DIM = LRU_WIDTH // LRU_BLOCKS
CONV_WIDTH = 4
CONV_PAD_LEFT = 2
CONV_PAD_RIGHT = 1
LRU_C = 8.0
D_FF = 3584
N_EXPERTS = 8
TOP_K = 2
N_EVEN = (DEPTH + 1) // 2
N_ODD = DEPTH // 2
ALPHA = (2 * DEPTH) ** 0.25
BETA = (8 * DEPTH) ** -0.25
LN_EPS = 1e-5
NEG_INF = -1e30

kernel_name = "hybrid_swa_rglru_moe_diffusion_trunk"


def layer_norm(h, g, b):
    hf = h.astype(jnp.float32)
    mu = jnp.mean(hf, axis=-1, keepdims=True)
    var = jnp.mean(jnp.square(hf - mu), axis=-1, keepdims=True)
    return ((hf - mu) * lax.rsqrt(var + LN_EPS) * g + b).astype(h.dtype)


def modulate(h, shift, scale):
    return h * (1 + scale) + shift


def grid_rope_angles(L):
    rows = L // GRID_W
    t = jnp.arange(rows * GRID_W)
    row = (t // GRID_W).astype(jnp.float32)
    col = (t % GRID_W).astype(jnp.float32)
    freqs = ROPE_BASE ** (-jnp.arange(0, AXIS_DIM, 2, dtype=jnp.float32) / AXIS_DIM)
    return row[:, None] * freqs, col[:, None] * freqs


def rotate_axis(u, ang):
    half = AXIS_DIM // 2
    cos = jnp.cos(ang)[None, :, None, :].astype(u.dtype)
    sin = jnp.sin(ang)[None, :, None, :].astype(u.dtype)
    u1, u2 = u[..., :half], u[..., half:]
    return jnp.concatenate([u1 * cos - u2 * sin, u2 * cos + u1 * sin], axis=-1)


def apply_rope_2d(u, row_ang, col_ang):
    return jnp.concatenate([rotate_axis(u[..., :AXIS_DIM], row_ang),
                            rotate_axis(u[..., AXIS_DIM:], col_ang)], axis=-1)


def windowed_gqa(hx, hc, w_qkv, w_o, sink, row_ang, col_ang, need_ctx):
    B, L, _ = hx.shape
    Lc = hc.shape[1]
    scale = HEAD_DIM ** -0.5

    def split_heads(u, n):
        q, k, v = jnp.split(u, [ATTN_DIM, ATTN_DIM + N_KV_HEADS * HEAD_DIM], axis=-1)
        return (q.reshape(B, n, N_HEADS, HEAD_DIM), k.reshape(B, n, N_KV_HEADS, HEAD_DIM),
                v.reshape(B, n, N_KV_HEADS, HEAD_DIM))

    q, k, v = split_heads(hx @ w_qkv, L)
    q = apply_rope_2d(q, row_ang, col_ang) * scale
    k = apply_rope_2d(k, row_ang, col_ang)
    qc, kc, vc = split_heads(hc @ w_qkv, Lc)
    sink_g = sink.reshape(N_KV_HEADS, GROUP).astype(jnp.float32)

    nb = L // BLOCK
    qb = q.reshape(B, nb, BLOCK, N_KV_HEADS, GROUP, HEAD_DIM)
    pad = ((0, 0), (BLOCK, BLOCK), (0, 0), (0, 0))
    kp = jnp.pad(k, pad).reshape(B, nb + 2, BLOCK, N_KV_HEADS, HEAD_DIM)
    vp = jnp.pad(v, pad).reshape(B, nb + 2, BLOCK, N_KV_HEADS, HEAD_DIM)
    k_band = jnp.concatenate([kp[:, :-2], kp[:, 1:-1], kp[:, 2:]], axis=2)
    v_band = jnp.concatenate([vp[:, :-2], vp[:, 1:-1], vp[:, 2:]], axis=2)
    s_loc = jnp.einsum('bnqkgd,bnjkd->bnkgqj', qb, k_band).astype(jnp.float32)
    qi = jnp.arange(BLOCK)[:, None]
    kj = jnp.arange(3 * BLOCK)[None, :]
    rel = kj - BLOCK - qi
    key_pos = jnp.arange(nb)[:, None, None] * BLOCK - BLOCK + kj[None]
    valid = (jnp.abs(rel) <= WINDOW)[None] & (key_pos >= 0) & (key_pos < L)
    s_loc = jnp.where(valid[None, :, None, None], s_loc, NEG_INF)
    s_ctx = jnp.einsum('bnqkgd,bckd->bnkgqc', qb, kc).astype(jnp.float32)
    s_sink = jnp.broadcast_to(sink_g[None, None, :, :, None, None],
                              (B, nb, N_KV_HEADS, GROUP, BLOCK, 1))
    p = jax.nn.softmax(jnp.concatenate([s_loc, s_ctx, s_sink], axis=-1), axis=-1).astype(hx.dtype)
    o = (jnp.einsum('bnkgqj,bnjkd->bnqkgd', p[..., :3 * BLOCK], v_band)
         + jnp.einsum('bnkgqc,bckd->bnqkgd', p[..., 3 * BLOCK:3 * BLOCK + Lc], vc))
    out_x = o.reshape(B, L, ATTN_DIM) @ w_o

    if need_ctx:
        qcs = qc.reshape(B, Lc, N_KV_HEADS, GROUP, HEAD_DIM) * scale
        sc = jnp.einsum('bqkgd,bckd->bkgqc', qcs, kc).astype(jnp.float32)
        sc_sink = jnp.broadcast_to(sink_g[None, :, :, None, None], (B, N_KV_HEADS, GROUP, Lc, 1))
        pc = jax.nn.softmax(jnp.concatenate([sc, sc_sink], axis=-1), axis=-1).astype(hc.dtype)
        oc = jnp.einsum('bkgqc,bckd->bqkgd', pc[..., :Lc], vc).reshape(B, Lc, ATTN_DIM)
        out_c = oc @ w_o
    else:
        out_c = None
    return out_x, out_c


def centred_dwconv(u, w, b):
    y = lax.conv_general_dilated(u, w[:, None, :].astype(u.dtype), window_strides=(1,),
                                 padding=[(CONV_PAD_LEFT, CONV_PAD_RIGHT)],
                                 dimension_numbers=('NWC', 'WIO', 'NWC'),
                                 feature_group_count=u.shape[-1])
    return y + b


def block_diag(u, w):
    ub = u.reshape(u.shape[:-1] + (LRU_BLOCKS, LRU_BLOCK_DIM))
    return jnp.einsum('blnd,nde->blne', ub, w).reshape(u.shape)


def linear_scan(a, b, h0):
    b = b.at[:, 0].add(a[:, 0] * h0)

    def combine(e1, e2):
        a1, b1 = e1
        a2, b2 = e2
        return a1 * a2, a2 * b1 + b2

    _, h = lax.associative_scan(combine, (a, b), axis=1)
    return h


def reverse_scan(a, b, h0):
    return jnp.flip(linear_scan(jnp.flip(a, axis=1), jnp.flip(b, axis=1), h0), axis=1)


def bidir_rglru(hx, hc, w_in, conv_w, conv_b, lam, w_r, b_r, w_i, b_i, w_out, need_ctx):
    B = hx.shape[0]
    R = LRU_WIDTH
    gate_x, xb = jnp.split(hx @ w_in, 2, axis=-1)
    if need_ctx:
        gate_c, xb_c = jnp.split(hc @ w_in, 2, axis=-1)
    else:
        xb_c = hc @ w_in[:, R:]
    u_x = centred_dwconv(xb, conv_w, conv_b)
    u_c = centred_dwconv(xb_c, conv_w, conv_b)

    def rg_lru_coeffs(u, d):
        r = jax.nn.sigmoid(block_diag(u, w_r[d]) + b_r[d]).astype(jnp.float32)
        ig = jax.nn.sigmoid(block_diag(u, w_i[d]) + b_i[d])
        log_a = -LRU_C * r * jax.nn.softplus(-lam[d].astype(jnp.float32))
        a = jnp.exp(log_a)
        bterm = jnp.sqrt(-jnp.expm1(2.0 * log_a)) * (ig * u).astype(jnp.float32)
        return a, bterm

    h0 = jnp.zeros((B, R), jnp.float32)
    a_c, b_c = rg_lru_coeffs(u_c, 0)
    hc_f = linear_scan(a_c, b_c, h0)
    a_x, b_x = rg_lru_coeffs(u_x, 0)
    hx_f = linear_scan(a_x, b_x, hc_f[:, -1])
    a_c, b_c = rg_lru_coeffs(u_c, 1)
    hc_b = reverse_scan(a_c, b_c, h0)
    a_x, b_x = rg_lru_coeffs(u_x, 1)
    hx_b = reverse_scan(a_x, b_x, hc_b[:, 0])

    out_x = (jax.nn.gelu(gate_x) * (hx_f + hx_b).astype(hx.dtype)) @ w_out
    if need_ctx:
        out_c = (jax.nn.gelu(gate_c) * (hc_f + hc_b).astype(hc.dtype)) @ w_out
    else:
        out_c = None
    return out_x, out_c


def swiglu(h, w_gu, w_dn):
    g, u = jnp.split(h @ w_gu, 2, axis=-1)
    return (jax.nn.silu(g) * u) @ w_dn


def moe_swiglu(h, router, w_gu, w_dn):
    logits = (h @ router).astype(jnp.float32)
    top_v, top_i = lax.top_k(logits, TOP_K)
    top_w = jax.nn.softmax(top_v, axis=-1)
    gates = jnp.einsum('blk,blke->ble', top_w,
                       jax.nn.one_hot(top_i, N_EXPERTS, dtype=jnp.float32)).astype(h.dtype)
    y = jnp.zeros_like(h)
    for e in range(N_EXPERTS):
        y = y + gates[..., e:e + 1] * swiglu(h, w_gu[e], w_dn[e])
    return y


def setup_inputs(seed: int = 0) -> dict:
    key = jax.random.key(seed)
    ks = iter(jax.random.split(key, 32))

    def nrm(shape, s):
        return jax.random.normal(next(ks), shape, jnp.float32) * s

    D, R, F = D_MODEL, LRU_WIDTH, D_FF
    a8 = jax.random.uniform(next(ks), (N_ODD, 2, R), jnp.float32, 0.9, 0.999)
    sig = a8 ** (1.0 / LRU_C)
    lru_lambda = jnp.log(sig) - jnp.log1p(-sig)
    return {
        "x": nrm((BATCH, SEQ, D), 1.0),
        "c": nrm((BATCH, D), 1.0),
        "ctx": nrm((BATCH, CTX_LEN, D), 1.0),
        "c_ctx": nrm((D,), 1.0),
        "w_mod": nrm((DEPTH, D, 6 * D), D ** -0.5),
        "b_mod": nrm((DEPTH, 6 * D), 0.02),
        "ln_g": 1.0 + nrm((DEPTH, 2, D), 0.02),
        "ln_b": nrm((DEPTH, 2, D), 0.02),
        "attn_w_qkv": nrm((N_EVEN, D, QKV_DIM), D ** -0.5),
        "attn_w_o": nrm((N_EVEN, ATTN_DIM, D), ATTN_DIM ** -0.5 * BETA),
        "attn_sink": nrm((N_EVEN, N_HEADS), 0.5),
        "ffn_w_gu": nrm((N_EVEN, D, 2 * F), D ** -0.5),
        "ffn_w_dn": nrm((N_EVEN, F, D), F ** -0.5 * BETA),
        "lru_w_in": nrm((N_ODD, D, 2 * R), D ** -0.5),
        "lru_conv_w": nrm((N_ODD, CONV_WIDTH, R), CONV_WIDTH ** -0.5),
        "lru_conv_b": nrm((N_ODD, R), 0.02),
        "lru_lambda": lru_lambda,
        "lru_w_r": nrm((N_ODD, 2, LRU_BLOCKS, LRU_BLOCK_DIM, LRU_BLOCK_DIM), LRU_BLOCK_DIM ** -0.5),
        "lru_b_r": nrm((N_ODD, 2, R), 0.02),
        "lru_w_i": nrm((N_ODD, 2, LRU_BLOCKS, LRU_BLOCK_DIM, LRU_BLOCK_DIM), LRU_BLOCK_DIM ** -0.5),
        "lru_b_i": nrm((N_ODD, 2, R), 0.02),
        "lru_w_out": nrm((N_ODD, R, D), R ** -0.5 * BETA),
        "moe_router": nrm((N_ODD, D, N_EXPERTS), D ** -0.5),
        "moe_w_gu": nrm((N_ODD, N_EXPERTS, D, 2 * F), D ** -0.5),
        "moe_w_dn": nrm((N_ODD, N_EXPERTS, F, D), F ** -0.5 * BETA),
    }


def reference(x, c, ctx, c_ctx, w_mod, b_mod, ln_g, ln_b,
              attn_w_qkv, attn_w_o, attn_sink, ffn_w_gu, ffn_w_dn,
              lru_w_in, lru_conv_w, lru_conv_b, lru_lambda, lru_w_r, lru_b_r, lru_w_i, lru_b_i,
              lru_w_out, moe_router, moe_w_gu, moe_w_dn):
    L = x.shape[1]
    Lc = ctx.shape[1]
    row_ang, col_ang = grid_rope_angles(L)
    silu_c = jax.nn.silu(c)
    silu_cc = jax.nn.silu(c_ctx)
    for i in range(DEPTH):
        need_ctx = i < DEPTH - 1
        j = i // N_MIXERS
        mod_x = (silu_c @ w_mod[i] + b_mod[i])[:, None, :]
        mod_c = (silu_cc @ w_mod[i] + b_mod[i])[None, None, :]
        sh1, sc1, g1, sh2, sc2, g2 = jnp.split(mod_x, 6, axis=-1)
        csh1, csc1, cg1, csh2, csc2, cg2 = jnp.split(mod_c, 6, axis=-1)

        hx = modulate(x, sh1, sc1)
        hc = modulate(ctx, csh1, csc1)
        if i % N_MIXERS == 0:
            ox, oc = windowed_gqa(hx, hc, attn_w_qkv[j], attn_w_o[j], attn_sink[j],
                                  row_ang, col_ang, need_ctx)
        else:
            ox, oc = bidir_rglru(hx, hc, lru_w_in[j], lru_conv_w[j], lru_conv_b[j], lru_lambda[j],
                                 lru_w_r[j], lru_b_r[j], lru_w_i[j], lru_b_i[j], lru_w_out[j],
                                 need_ctx)
        x = layer_norm(ALPHA * x + g1 * ox, ln_g[i, 0], ln_b[i, 0])

        hx = modulate(x, sh2, sc2)
        if need_ctx:
            ctx = layer_norm(ALPHA * ctx + cg1 * oc, ln_g[i, 0], ln_b[i, 0])
            h = jnp.concatenate([modulate(ctx, csh2, csc2), hx], axis=1)
        else:
            h = hx
        if i % 2 == 0:
            f = swiglu(h, ffn_w_gu[j], ffn_w_dn[j])
        else:
            f = moe_swiglu(h, moe_router[j], moe_w_gu[j], moe_w_dn[j])
        if need_ctx:
            ctx = layer_norm(ALPHA * ctx + cg2 * f[:, :Lc], ln_g[i, 1], ln_b[i, 1])
            fx = f[:, Lc:]
        else:
            fx = f
        x = layer_norm(ALPHA * x + g2 * fx, ln_g[i, 1], ln_b[i, 1])
    return x
```

```python
import contextlib
import numpy as np
import concourse.bass as bass
import concourse.mybir as mybir
from concourse.bass_utils import run_bass_kernel_spmd

F32 = mybir.dt.float32
BF16 = mybir.dt.bfloat16
AF = mybir.ActivationFunctionType
ALU = mybir.AluOpType
AX = mybir.AxisListType

D = 1024
L = 4096
LC = 256
T = L + LC
NT = T // 128
FF = 3584
NE = 8
NG = 7
ALPHA = 4.0 ** 0.25
EPS = 1e-5
QSCALE = 128.0 ** -0.5
NQ = 4355

ENGS = ["pe", "act", "dve", "pool", "sp"]
DMA_RING = 8


class Tok:
    __slots__ = ("w", "rs")

    def __init__(self):
        self.w = None
        self.rs = []


class Op:
    __slots__ = ("eng", "fn", "is_dma", "deps", "inc", "ticket", "dma_sem", "dma_val", "waits", "free")

    def __init__(self, eng, fn, is_dma):
        self.eng = eng
        self.fn = fn
        self.is_dma = is_dma
        self.deps = []
        self.inc = False
        self.ticket = None
        self.dma_sem = None
        self.dma_val = None
        self.waits = []
        self.free = False


class Prog:
    def __init__(self, nc):
        self.nc = nc
        self.ops = {e: [] for e in ENGS}
        self.ndma = {e: 0 for e in ENGS}
        self.dma_ops = {e: [] for e in ENGS}
        self.last_real = {e: None for e in ENGS}

    def _add(self, op, reads, writes):
        deps = op.deps
        for r in reads:
            if r.w is not None:
                deps.append(r.w)
        for w in writes:
            if w.w is not None:
                deps.append(w.w)
            deps.extend(w.rs)
        for w in writes:
            w.w = op
            w.rs = []
        for r in reads:
            if not op.is_dma:
                r.rs = [o for o in r.rs if o.is_dma or o.eng != op.eng]
            r.rs.append(op)
        self.ops[op.eng].append(op)
        if op.fn is not None and not op.is_dma:
            self.last_real[op.eng] = op
        return op

    def op(self, eng, fn, reads=(), writes=()):
        return self._add(Op(eng, fn, False), reads, writes)

    def dma(self, eng, fn, reads=(), writes=(), free=False):
        op = Op(eng, fn, True)
        op.free = free
        k = self.ndma[eng]
        self.ndma[eng] += 1
        op.dma_sem = k % DMA_RING
        op.dma_val = 16 * (k // DMA_RING + 1)
        if k >= DMA_RING:
            op.deps.append(self.dma_ops[eng][k - DMA_RING])
        self.dma_ops[eng].append(op)
        return self._add(op, reads, writes)

    def wait(self, eng, toks):
        return self._add(Op(eng, None, False), toks, ())

    def barrier(self):
        deps = [o for o in self.last_real.values() if o is not None]
        for e in ENGS:
            deps.extend(o for o in self.dma_ops[e][-DMA_RING:] if not o.free)
        for e in ENGS:
            if e == "pool":
                continue
            op = Op(e, None, False)
            op.deps = list(deps)
            self.ops[e].append(op)

    def finalize(self):
        for e in ENGS:
            for op in self.ops[e]:
                nd = []
                for d in op.deps:
                    if d is op or d.fn is None:
                        continue
                    if d.is_dma:
                        nd.append(d)
                        continue
                    if d.eng == op.eng and op.eng == "pe" and not op.is_dma:
                        continue
                    d.inc = True
                    nd.append(d)
                op.deps = nd
        for e in ENGS:
            c = 0
            for op in self.ops[e]:
                if op.inc:
                    c += 1
                    op.ticket = c
        for e in ENGS:
            waited = {}
            for op in self.ops[e]:
                need = {}
                for d in op.deps:
                    if d.is_dma:
                        key = ("dma", d.eng, d.dma_sem)
                        val = d.dma_val
                    else:
                        key = ("eng", d.eng)
                        val = d.ticket
                    if val > need.get(key, 0):
                        need[key] = val
                for key, val in need.items():
                    if val > waited.get(key, 0):
                        waited[key] = val
                        op.waits.append((key, val))

    def emit(self, stack):
        nc = self.nc
        self.finalize()
        sems = {}
        for e in ENGS:
            sems[("eng", e)] = stack.enter_context(nc.semaphore(f"p_{e}"))
            if self.ndma[e]:
                for k in range(DMA_RING):
                    sems[("dma", e, k)] = stack.enter_context(nc.semaphore(f"d_{e}{k}"))
        block = stack.enter_context(nc.Block())

        def run(e):
            def body(engine):
                for op in self.ops[e]:
                    for key, val in op.waits:
                        engine.wait_ge(sems[key], val)
                    if op.fn is None:
                        continue
                    ins = op.fn(engine)
                    if op.is_dma:
                        ins.then_inc(sems[("dma", e, op.dma_sem)], 16)
                    elif op.inc:
                        ins.then_inc(sems[("eng", e)], 1)
            return body

        block.tensor(run("pe"))
        block.scalar(run("act"))
        block.vector(run("dve"))
        block.gpsimd(run("pool"))
        block.sync(run("sp"))

    def mm(self, out, lhsT, rhs, start, stop, r, w):
        return self.op("pe", lambda e: e.matmul(out, lhsT, rhs, start=start, stop=stop), r, w)

    def tr(self, out, in_, ident, r, w):
        return self.op("pe", lambda e: e.transpose(out, in_, ident), r, w)

    def act(self, out, in_, func, r, w, bias=None, scale=None, accum_out=None):
        kw = {}
        if bias is not None:
            kw["bias"] = bias
        if scale is not None:
            kw["scale"] = scale
        if accum_out is not None:
            kw["accum_out"] = accum_out
        return self.op("act", lambda e: e.activation(out=out, in_=in_, func=func, **kw), r, w)

    def tt(self, out, in0, in1, op, r, w, eng="dve"):
        return self.op(eng, lambda e: e.tensor_tensor(out=out, in0=in0, in1=in1, op=op), r, w)

    def ts(self, out, in0, s1, s2, op0, op1, r, w, eng="dve", accum_out=None):
        kw = {}
        if accum_out is not None:
            kw["accum_out"] = accum_out
        if op1 is None:
            return self.op(eng, lambda e: e.tensor_scalar(out=out, in0=in0, scalar1=s1, scalar2=None, op0=op0, **kw), r, w)
        return self.op(eng, lambda e: e.tensor_scalar(out=out, in0=in0, scalar1=s1, scalar2=s2, op0=op0, op1=op1, **kw), r, w)

    def stt(self, out, in0, scalar, in1, op0, op1, r, w, accum_out=None):
        kw = {}
        if accum_out is not None:
            kw["accum_out"] = accum_out
        return self.op("dve", lambda e: e.scalar_tensor_tensor(out=out, in0=in0, scalar=scalar, in1=in1, op0=op0, op1=op1, **kw), r, w)

    def cp(self, out, in_, r, w, eng="dve"):
        if eng == "act":
            return self.act(out, in_, AF.Copy, r, w)
        return self.op(eng, lambda e: e.tensor_copy(out=out, in_=in_), r, w)

    def ld(self, out, in_, r, w, eng="sp", free=False, slow=False):
        if slow:
            return self.dma(eng, lambda e: e.dma_start(out=out, in_=in_, allow_slow_non_contiguous=True), r, w, free=free)
        return self.dma(eng, lambda e: e.dma_start(out=out, in_=in_), r, w, free=free)


class Arena:
    def __init__(self, nc, st, nbytes):
        self.t = st.enter_context(nc.sbuf_tensor("arena", [128, nbytes // 4], F32))
        self.top = 0
        self.cap = nbytes
        self.hi = 0

    def alloc(self, shape, dtype, parts=128):
        n = int(np.prod(shape))
        esz = 2 if dtype == BF16 else 4
        nb = (n * esz + 63) // 64 * 64
        off = self.top
        self.top += nb
        self.hi = max(self.hi, self.top)
        assert self.top <= self.cap, f"arena overflow {self.top} > {self.cap}"
        ap = self.t[0:parts, off // 4:(off + nb) // 4]
        if esz == 2:
            ap = ap.bitcast(BF16)
        ap = ap[:, 0:n]
        if len(shape) == 2:
            ap = ap.rearrange("p (a b) -> p a b", a=shape[0])
        elif len(shape) == 3:
            ap = ap.rearrange("p (a b c) -> p a b c", a=shape[0], b=shape[1])
        elif len(shape) == 4:
            ap = ap.rearrange("p (a b c d) -> p a b c d", a=shape[0], b=shape[1], c=shape[2])
        return ap


def bcast_rows(ap_row, n, parts=128):
    return bass.AP(ap_row.tensor, ap_row.offset, [[0, parts], [1, n]])


def rev(ap2d):
    (ps, pn), (fs, fn) = ap2d.ap
    return bass.AP(ap2d.tensor, ap2d.offset + fs * (fn - 1), [[ps, pn], [-fs, fn]])


def build(dbg=None):
    nc = bass.Bass("TRN2", target_bir_lowering=False)

    def din(name, shape, dt=F32):
        return nc.dram_tensor(name, list(shape), dt, kind="ExternalInput").ap()

    def dscr(name, shape, dt=F32):
        kind = "ExternalOutput" if (dbg and name in dbg) else "Internal"
        return nc.dram_tensor(name, list(shape), dt, kind=kind).ap()

    x_in = din("x", [L, D])
    ctx_in = din("ctx", [LC, D])
    cvec = din("cvec", [2, D])
    w_mod = din("w_mod", [2, D, 6 * D])
    b_mod = din("b_mod", [2, 6 * D])
    ln_g = din("ln_g", [2, 2, D])
    ln_b = din("ln_b", [2, 2, D])
    a_wqkv = din("attn_w_qkv", [D, 1536])
    a_wo = din("attn_w_o", [D, D])
    a_sink = din("attn_sink", [1, 8])
    f_wgu = din("ffn_w_gu", [D, 2 * FF])
    f_wdn = din("ffn_w_dn", [FF, D])
    l_win = din("lru_w_in", [D, 2 * D])
    l_cw = din("lru_conv_w", [4, D])
    l_cb = din("lru_conv_b", [1, D])
    l_lam = din("lru_lambda", [2, D])
    l_wr = din("lru_w_r", [2 * 8 * 128, 128])
    l_br = din("lru_b_r", [2, D])
    l_wi = din("lru_w_i", [2 * 8 * 128, 128])
    l_bi = din("lru_b_i", [2, D])
    l_wout = din("lru_w_out", [D, D])
    m_router = din("moe_router", [D, NE])
    m_wgu = din("moe_w_gu", [NE, D, 2 * FF])
    m_wdn = din("moe_w_dn", [NE, FF, D])
    c_ident = din("c_ident", [128, 128])
    c_cos = din("c_cos", [128, L])
    c_sin = din("c_sin", [128, L])
    c_mask = din("c_mask", [128, 3 * 384])
    out = nc.dram_tensor("out", [L, D], F32, kind="ExternalOutput").ap()

    modrow = dscr("modrow", [2, 2, 6 * D])
    xs1 = dscr("xs1", [T, D])
    xs2 = dscr("xs2", [T, D])
    xs3 = dscr("xs3", [L, D])
    wqkv_s = dscr("wqkv_s", [D, 1536], BF16)
    wo_s = dscr("wo_s", [D, D], BF16)
    wgu_s = dscr("wgu_s", [1 + NE, NG, D, 1024], BF16)
    wdn_s = dscr("wdn_s", [1 + NE, FF, D], BF16)
    win_s = dscr("win_s", [D, 2 * D], BF16)
    wout_s = dscr("wout_s", [D, D], BF16)
    wr_s = dscr("wr_s", [2048, 128], BF16)
    wi_s = dscr("wi_s", [2048, 128], BF16)
    xb_s = dscr("xb_s", [8, 128, T])
    gate_s = dscr("gate_s", [8, 128, L], BF16)
    y_s = dscr("y_s", [8, 128, L], BF16)

    st = contextlib.ExitStack()
    with st:
        P = Prog(nc)
        AR = Arena(nc, st, 206 * 1024)
        psum = st.enter_context(nc.psum_tensor("psum", [128, 8, 512], F32))
        pb = [psum[:, i, :] for i in range(8)]
        pbt = [Tok() for _ in range(8)]

        def psum2(i):
            return psum[:, i:i + 2, :].rearrange("p a b -> p (a b)")

        t_wqkv, t_wo, t_win, t_wout, t_wr, t_wi = Tok(), Tok(), Tok(), Tok(), Tok(), Tok()
        t_wgu = [[Tok() for _ in range(NG)] for _ in range(1 + NE)]
        t_wdn = [Tok() for _ in range(1 + NE)]

        def cast(dst, src, tok):
            P.ld(dst, src, [], tok if isinstance(tok, list) else [tok], eng="pool", free=True)

        def cast_ffn(idx, wgu_src, wdn_src):
            dstv = wgu_s[idx].rearrange("g d (u n) -> d u g n", u=2)
            srcv = wgu_src.rearrange("d (u g n) -> d u g n", u=2, g=NG)
            for q in range(4):
                P.ld(dstv[q * 256:(q + 1) * 256], srcv[q * 256:(q + 1) * 256], [], t_wgu[idx], eng="pool", free=True)
            for q in range(2):
                P.ld(wdn_s[idx][q * 1792:(q + 1) * 1792, :], wdn_src[q * 1792:(q + 1) * 1792, :], [], [t_wdn[idx]], eng="pool", free=True)

        cast(wqkv_s, a_wqkv, t_wqkv)
        cast(wo_s, a_wo, t_wo)
        if dbg is None or dbg.get("ffn", True):
            cast_ffn(0, f_wgu, f_wdn)
        cast(win_s, l_win, t_win)
        cast(wout_s, l_wout, t_wout)
        cast(wr_s, l_wr, t_wr)
        cast(wi_s, l_wi, t_wi)
        if dbg is None or dbg.get("moe", True):
            for e in range(NE):
                cast_ffn(1 + e, m_wgu[e], m_wdn[e])

        ident = AR.alloc([128], F32)
        identb = AR.alloc([128], BF16)
        modT = AR.alloc([2, 96], F32)
        lruT = AR.alloc([120], F32)
        sinkb = AR.alloc([8], F32)
        t_ident, t_modT, t_lruT, t_sink = Tok(), Tok(), Tok(), Tok()
        P.ld(ident, c_ident, [], [t_ident])
        P.cp(identb, ident, [t_ident], [t_ident])
        P.ld(sinkb, bass.AP(a_sink.tensor, a_sink.offset, [[0, 128], [1, 8]]), [], [t_sink])
        base_mark = AR.top

        t_modrow = Tok()
        if True:
            cv = AR.alloc([2, 8], F32)
            cvs = AR.alloc([2, 8], F32)
            bmod_sb = AR.alloc([2 * 6 * D], F32)
            mrow = AR.alloc([2 * 6 * D], F32)
            wch = [AR.alloc([8, 512], F32) for _ in range(3)]
            mt_in = AR.alloc([2, 128], F32)
            lst = AR.alloc([128], F32)
            t_cv, t_bm, t_mrow, t_mtin, t_lst = Tok(), Tok(), Tok(), Tok(), Tok()
            t_wch = [Tok() for _ in range(3)]
            for s_ in range(2):
                P.ld(cv[:, s_, :], cvec[s_].rearrange("(c p) -> p c", p=128), [], [t_cv], slow=True)
            P.act(cvs, cv, AF.Silu, [t_cv], [t_cv])
            P.ld(bmod_sb[0:2, :], bass.AP(b_mod.tensor, b_mod.offset, [[0, 2], [1, 2 * 6 * D]]), [], [t_bm])
            k = 0
            for i in range(2):
                wmv = w_mod[i].rearrange("(c p) n -> p c n", p=128)
                for n in range(12):
                    s = k % 3
                    k += 1
                    P.ld(wch[s], wmv[:, :, n * 512:(n + 1) * 512], [], [t_wch[s]])
                    for c in range(8):
                        P.mm(pb[0][0:2, :], cvs[:, :, c], wch[s][:, c, :], c == 0, c == 7, [t_cv, t_wch[s]], [pbt[0]])
                    o = i * 6 * D + n * 512
                    P.tt(mrow[0:2, o:o + 512], pb[0][0:2, :], bmod_sb[0:2, o:o + 512], ALU.add, [pbt[0], t_bm], [t_mrow])
            P.ld(modrow.rearrange("s i n -> s (i n)"), mrow[0:2, :], [t_mrow], [t_modrow])
            P.ld(mt_in[0:96], bass.AP(modrow.tensor, modrow.offset, [[128, 96], [2 * 6 * D, 2], [1, 128]]), [t_modrow], [t_mtin])
            for s in range(2):
                P.tr(pb[1][:, 0:96], mt_in[0:96, s, :], ident[0:96, 0:96], [t_mtin, t_ident], [pbt[1]])
                P.cp(modT[:, s, :], pb[1][:, 0:96], [pbt[1]], [t_modT])
            for s in range(2):
                for i in range(2):
                    for m in (1, 4):
                        sl = modT[:, s, i * 48 + m * 8:i * 48 + m * 8 + 8]
                        P.ts(sl, sl, 1.0, None, ALU.add, None, [], [t_modT])
            P.ld(lst[0:32], l_cw.rearrange("j (c p) -> (j c) p", p=128), [], [t_lst])
            P.ld(lst[32:40], l_cb.rearrange("j (c p) -> (j c) p", p=128), [], [t_lst])
            P.ld(lst[40:56], l_lam.rearrange("j (c p) -> (j c) p", p=128), [], [t_lst])
            P.ld(lst[56:72], l_br.rearrange("j (c p) -> (j c) p", p=128), [], [t_lst])
            P.ld(lst[72:88], l_bi.rearrange("j (c p) -> (j c) p", p=128), [], [t_lst])
            P.tr(pb[2][:, 0:88], lst[0:88, :], ident[0:88, 0:88], [t_lst, t_ident], [pbt[2]])
            P.cp(lruT[:, 0:88], pb[2][:, 0:88], [pbt[2]], [t_lruT])
            P.act(lruT[:, 88:104], lruT[:, 40:56], AF.Exp, [t_lruT], [t_lruT], scale=-1.0)
            P.act(lruT[:, 88:104], lruT[:, 88:104], AF.Ln, [t_lruT], [t_lruT], bias=1.0)
            P.ts(lruT[:, 104:120], lruT[:, 88:104], -16.0, None, ALU.mult, None, [t_lruT], [t_lruT])
            P.ts(lruT[:, 88:104], lruT[:, 88:104], -8.0, None, ALU.mult, None, [t_lruT], [t_lruT])
        P.barrier()
        AR.top = base_mark

        def modv(s, i, m, c):
            j = i * 48 + m * 8 + c
            return modT[:, s, j:j + 1]

        def load_rows(dst, rowap, tok):
            P.ld(dst, bcast_rows(rowap, D), [t_modrow], [tok])

        class LNWork:
            def __init__(self, nslots=2):
                self.n = nslots
                self.xres = [AR.alloc([D], F32) for _ in range(nslots)]
                self.v = [AR.alloc([D], F32) for _ in range(nslots)]
                self.st = [AR.alloc([16], F32) for _ in range(nslots)]
                self.tx = [Tok() for _ in range(nslots)]
                self.tv = [Tok() for _ in range(nslots)]
                self.ts_ = [Tok() for _ in range(nslots)]
                self.k = 0

        def ln_epilogue(W, o_halves, o_toks, xres_src, gate_row, lng, lnb, row_toks, dst, xres_toks=()):
            s = W.k % W.n
            W.k += 1
            xres, v, stt_ = W.xres[s], W.v[s], W.st[s]
            tx, tv, tst = W.tx[s], W.tv[s], W.ts_[s]
            P.ld(xres, xres_src, list(xres_toks), [tx])
            for h in range(2):
                P.tt(v[:, h * 512:(h + 1) * 512], o_halves[h], gate_row[:, h * 512:(h + 1) * 512], ALU.mult,
                     list(o_toks) + list(row_toks), [tv])
            P.stt(v, xres, ALPHA, v, ALU.mult, ALU.add, [tx], [tv])
            for h in range(2):
                P.op("dve", lambda e, h=h: e.bn_stats(out=stt_[:, h * 6:(h + 1) * 6], in_=v[:, h * 512:(h + 1) * 512]), [tv], [tst])
            P.op("dve", lambda e: e.bn_aggr(out=stt_[:, 12:14], in_=stt_[:, 0:12]), [], [tst])
            P.act(stt_[:, 14:15], stt_[:, 13:14], AF.Ln, [t_eps], [tst], bias=EPS_AP[0])
            P.act(stt_[:, 14:15], stt_[:, 14:15], AF.Exp, [], [tst], scale=-0.5)
            P.ts(stt_[:, 15:16], stt_[:, 12:13], stt_[:, 14:15], -1.0, ALU.mult, ALU.mult, [], [tst])
            P.act(xres, v, AF.Identity, [tv, tst], [tx], bias=stt_[:, 15:16], scale=stt_[:, 14:15])
            P.tt(xres, xres, lng, ALU.mult, list(row_toks), [tx])
            P.tt(xres, xres, lnb, ALU.add, list(row_toks), [tx])
            P.ld(dst, xres, [tx], dst_tok_of(dst))

        dst_toks = {}

        def dst_tok_of(dst):
            key = dst.tensor.name
            if key not in dst_toks:
                dst_toks[key] = Tok()
            return [dst_toks[key]]

        eps_t = AR.alloc([1], F32)
        t_eps = Tok()
        P.op("dve", lambda e: e.memset(eps_t, EPS), [], [t_eps])
        EPS_AP = [eps_t[:, 0:1]]
        base_mark = AR.top

        class TPWork:
            def __init__(self, nslots=2):
                self.n = nslots
                self.xt = [AR.alloc([D], F32) for _ in range(nslots)]
                self.t = [Tok() for _ in range(nslots)]
                self.k = 0

        def mod_transpose(W, src, src_toks, s, i, msh, msc, dst_fn, dst_tok, banks=(0, 1), dst2_fn=None, dst2_tok=None):
            sl = W.k % W.n
            W.k += 1
            xt, tx = W.xt[sl], W.t[sl]
            P.ld(xt, src, list(src_toks), [tx])
            for c in range(8):
                bk = banks[c // 4]
                P.tr(pb[bk][:, (c % 4) * 128:(c % 4 + 1) * 128], xt[:, c * 128:(c + 1) * 128], ident, [tx, t_ident], [pbt[bk]])
            for c in range(8):
                bk = banks[c // 4]
                src_ps = pb[bk][:, (c % 4) * 128:(c % 4 + 1) * 128]
                sc, sh = modv(s, i, msc, c), modv(s, i, msh, c)
                if c % 2 == 0:
                    P.act(dst_fn(c), src_ps, AF.Identity, [pbt[bk], t_modT], [dst_tok], bias=sh, scale=sc)
                else:
                    P.ts(dst_fn(c), src_ps, sc, sh, ALU.mult, ALU.add, [pbt[bk], t_modT], [dst_tok])
                if dst2_fn is not None and not (dbg or {}).get("no_dst2"):
                    if c % 2 == 1:
                        P.act(dst2_fn(c), src_ps, AF.Identity, [pbt[bk], t_modT], [dst2_tok], bias=sh, scale=sc)
                    else:
                        P.ts(dst2_fn(c), src_ps, sc, sh, ALU.mult, ALU.add, [pbt[bk], t_modT], [dst2_tok])

        def tile_src(tensor_lat, tensor_ctx, t):
            if t < 2:
                return tensor_ctx[t * 128:(t + 1) * 128, :]
            return tensor_lat[(t - 2) * 128:(t - 1) * 128, :]

        if dbg is None or dbg.get("attn", True):
            wq = AR.alloc([8, 1536], BF16)
            wqp = AR.alloc([8, 1280], BF16)
            wo = AR.alloc([8, 1024], BF16)
            csr = [AR.alloc([2, 128], F32) for _ in range(2)]
            t_csr = [Tok(), Tok()]
            maskf = AR.alloc([3 * 384], F32)
            maskb = AR.alloc([3, 384], BF16)
            KT = AR.alloc([2, 36 * 128], BF16)
            V = AR.alloc([36, 256], BF16)
            qT = [AR.alloc([8, 128], BF16) for _ in range(2)]
            hT = [AR.alloc([8, 128], BF16) for _ in range(2)]
            oT = [AR.alloc([8, 128], BF16) for _ in range(2)]
            pexp = [AR.alloc([648], BF16) for _ in range(2)]
            ones_f = AR.alloc([128], F32)
            sinkq = AR.alloc([8], F32)
            pn = [AR.alloc([640], BF16) for _ in range(2)]
            PT = [AR.alloc([640], BF16) for _ in range(2)]
            rt1 = [AR.alloc([128], F32) for _ in range(2)]
            rt2 = [AR.alloc([128], F32) for _ in range(2)]
            sm = [AR.alloc([8], F32) for _ in range(2)]
            g1x, g1c, lng, lnb = (AR.alloc([D], F32) for _ in range(4))
            t_wq, t_wqp, t_wos, t_cs, t_mask, t_rows = Tok(), Tok(), Tok(), Tok(), Tok(), Tok()
            t_K = [Tok() for _ in range(36)]
            t_V = [Tok() for _ in range(36)]
            t_q = [Tok(), Tok()]
            t_h = [Tok(), Tok()]
            t_o = [Tok(), Tok()]
            t_pe = [Tok(), Tok()]
            t_pn = [Tok(), Tok()]
            t_PT = [Tok(), Tok()]
            t_r1 = [Tok(), Tok()]
            t_r2 = [Tok(), Tok()]
            t_sm = [Tok(), Tok()]
            t_S = [Tok(), Tok()]
            t_pt = [Tok(), Tok()]
            t_pv = t_pt
            t_sq = Tok()
            TW = TPWork(2)
            LW = LNWork(2)
            P.op("dve", lambda e: e.memset(ones_f, 1.0), [], [t_sq])
            P.ts(sinkq, sinkb, float(128.0 ** 0.5), None, ALU.mult, None, [t_sink], [t_sq])

            P.ld(wq, wqkv_s.rearrange("(c p) n -> p c n", p=128), [t_wqkv], [t_wq])
            P.ld(wo, wo_s.rearrange("(c p) n -> p c n", p=128), [t_wo], [t_wos])
            wv = wq[:, :, 0:1280].rearrange("p c (h f s) -> p c h f s", f=2, s=32)
            wpv = wqp.rearrange("p c (h f s) -> p c h f s", f=2, s=32)
            for c in range(8):
                P.cp(wpv[:, c, :, 0, :], wv[:, c, :, 1, :], [t_wq], [t_wqp])
                P.cp(wpv[:, c, :, 1, :], wv[:, c, :, 0, :], [t_wq], [t_wqp], eng="act")
            P.ld(maskf, c_mask, [], [t_mask])
            P.ts(maskb.rearrange("p a b -> p (a b)"), maskf, -1.0, 30000.0, ALU.add, ALU.mult, [t_mask], [t_mask])
            load_rows(g1x, modrow[0, 0, 2 * D:3 * D], t_rows)
            load_rows(g1c, modrow[1, 0, 2 * D:3 * D], t_rows)
            load_rows(lng, ln_g[0, 0, :], t_rows)
            load_rows(lnb, ln_b[0, 0, :], t_rows)
            for blk in (2, 35):
                for kh in range(2):
                    P.op("dve", lambda e, blk=blk, kh=kh: e.memset(KT[:, kh, blk * 128:(blk + 1) * 128], 0.0), [], [t_K[blk]])
                P.op("dve", lambda e, blk=blk: e.memset(V[:, blk, :], 0.0), [], [t_V[blk]])

            def blk_of(t):
                return t if t < 2 else t + 1

            t_pa, t_pb = Tok(), Tok()

            def proj(t):
                sl = t % 2
                s = 1 if t < 2 else 0
                lat = t >= 2
                pos = (t - 2) * 128
                blk = blk_of(t)
                mod_transpose(TW, tile_src(x_in, ctx_in, t), [], s, 0, 0, 1, lambda c: hT[sl][:, c, :], t_h[sl])
                if lat:
                    P.ld(csr[sl][:, 0, :], c_cos[:, pos:pos + 128], [], [t_csr[sl]])
                    P.ld(csr[sl][:, 1, :], c_sin[:, pos:pos + 128], [], [t_csr[sl]])
                for j, hcol in enumerate([8, 9]):
                    for c in range(8):
                        P.mm(pb[0][:, j * 128:(j + 1) * 128], wq[:, c, hcol * 128:(hcol + 1) * 128], hT[sl][:, c, :],
                             c == 0, c == 7, [t_wq, t_h[sl]], [pbt[0]])
                    if lat:
                        for c in range(8):
                            P.mm(pb[1][:, j * 128:(j + 1) * 128], wqp[:, c, hcol * 128:(hcol + 1) * 128], hT[sl][:, c, :],
                                 c == 0, c == 7, [t_wqp, t_h[sl]], [pbt[1]])
                for j, hcol in enumerate([8, 9]):
                    dstap, dtok = KT[:, hcol - 8, blk * 128:(blk + 1) * 128], t_K[blk]
                    a_ps = pb[0][:, j * 128:(j + 1) * 128]
                    if lat:
                        b_ps = pb[1][:, j * 128:(j + 1) * 128]
                        r1, r2 = rt1[j % 2], rt2[j % 2]
                        P.tt(r1, a_ps, csr[sl][:, 0, :], ALU.mult, [pbt[0], t_csr[sl]], [t_r1[j % 2]])
                        P.tt(r2, b_ps, csr[sl][:, 1, :], ALU.mult, [pbt[1], t_csr[sl]], [t_r2[j % 2]])
                        P.tt(dstap, r1, r2, ALU.add, [t_r1[j % 2], t_r2[j % 2]], [dtok])
                    else:
                        P.cp(dstap, a_ps, [pbt[0]], [dtok], eng="act")
                for c in range(8):
                    P.mm(pb[0][:, 256:512], hT[sl][:, c, :], wq[:, c, 1280:1536], c == 0, c == 7, [t_wq, t_h[sl]], [pbt[0]])
                P.cp(V[:, blk, :], pb[0][:, 256:512], [pbt[0]], [t_V[blk]], eng="act")

            def proj_q_head(t, h):
                sl = t % 2
                lat = t >= 2
                a_ps, b_ps = pb[0][:, 0:128], pb[1][:, 0:128]
                t_pa, t_pb = pbt[0], pbt[1]
                for c in range(8):
                    P.mm(a_ps, wq[:, c, h * 128:(h + 1) * 128], hT[sl][:, c, :], c == 0, c == 7, [t_wq, t_h[sl]], [t_pa])
                if lat:
                    for c in range(8):
                        P.mm(b_ps, wqp[:, c, h * 128:(h + 1) * 128], hT[sl][:, c, :], c == 0, c == 7, [t_wqp, t_h[sl]], [t_pb])
                    r1, r2 = rt1[h % 2], rt2[h % 2]
                    P.tt(r1, a_ps, csr[sl][:, 0, :], ALU.mult, [t_pa, t_csr[sl]], [t_r1[h % 2]])
                    P.tt(r2, b_ps, csr[sl][:, 1, :], ALU.mult, [t_pb, t_csr[sl]], [t_r2[h % 2]])
                    P.tt(qT[sl][:, h, :], r1, r2, ALU.add, [t_r1[h % 2], t_r2[h % 2]], [t_q[sl]])
                else:
                    P.cp(qT[sl][:, h, :], a_ps, [t_pa], [t_q[sl]], eng="act")

            S = [psum2(2), psum2(4)]
            pTb = [pb[6].bitcast(BF16), pb[7].bitcast(BF16)]
            PVr = [pb[6][:, 384:512], pb[7][:, 384:512]]

            def attn(t, fill=None):
                sl = t % 2
                lat = t >= 2
                blk = blk_of(t)
                if lat:
                    n = t - 2
                    mv = 0 if n == 0 else (2 if n == 31 else 1)
                    lo, width = 128, 640
                    kblocks = [blk - 1, blk, blk + 1, 0, 1]
                else:
                    mv = 1
                    lo, width = 512, 256
                    kblocks = [0, 1]
                nk = len(kblocks)
                wd = nk * 128

                def s_mm(h):
                    u, kh = h % 2, h // 4
                    Su = S[u]
                    if lat:
                        P.mm(Su[:, 128:512], qT[sl][:, h, :], KT[:, kh, (blk - 1) * 128:(blk + 2) * 128], True, False,
                             [t_q[sl], t_K[blk - 1], t_K[blk], t_K[blk + 1]], [t_S[u]])
                        P.mm(Su[:, 128:512], identb, maskb[:, mv, :], False, True, [t_ident, t_mask], [t_S[u]])
                    P.mm(Su[:, 512:768], qT[sl][:, h, :], KT[:, kh, 0:256], True, True, [t_q[sl], t_K[0], t_K[1]], [t_S[u]])
                    P.mm(Su[:, 768:769], ones_f[0:1, :], sinkq[0:1, h:h + 1], True, True, [t_sq], [t_S[u]])

                def soft(h):
                    u = h % 2
                    Su, smt, tsm = S[u], sm[u], t_sm[u]
                    pe_, pn_ = pexp[u], pn[u]
                    P.op("dve", lambda e: e.tensor_reduce(out=smt[:, 0:1], in_=Su[:, lo:769], axis=AX.X, op=ALU.max, negate=True),
                         [t_S[u]], [tsm])
                    P.ts(smt[:, 1:2], smt[:, 0:1], QSCALE, None, ALU.mult, None, [], [tsm])
                    P.act(pe_[:, 0:width + 1], Su[:, lo:769], AF.Exp, [t_S[u], tsm], [t_pe[u], tsm], bias=smt[:, 1:2], scale=QSCALE,
                          accum_out=smt[:, 2:3])
                    P.op("dve", lambda e: e.reciprocal(out=smt[:, 3:4], in_=smt[:, 2:3]), [], [tsm])
                    P.ts(pn_[:, 0:wd], pe_[:, 0:wd], smt[:, 3:4], None, ALU.mult, None, [t_pe[u], tsm], [t_pn[u]])

                def p_tr(h):
                    u = h % 2
                    for j in range(nk):
                        P.tr(pTb[u][:, j * 128:(j + 1) * 128], pn[u][:, j * 128:(j + 1) * 128], identb, [t_pn[u], t_ident], [t_pt[u]])
                    P.cp(PT[u][:, 0:wd], pTb[u][:, 0:wd], [t_pt[u]], [t_PT[u]], eng="act")

                def p_v(h):
                    u, kh = h % 2, h // 4
                    for j, kb in enumerate(kblocks):
                        P.mm(PVr[u], V[:, kb, kh * 128:(kh + 1) * 128], PT[u][:, j * 128:(j + 1) * 128],
                             j == 0, j == nk - 1, [t_V[kb], t_PT[u]], [t_pv[u]])
                    P.cp(oT[sl][:, h, :], PVr[u], [t_pv[u]], [t_o[sl]])

                s_mm(0)
                soft(0)
                s_mm(1)
                for h in range(8):
                    if h + 2 < 8:
                        s_mm(h + 2)
                    p_tr(h)
                    if h + 1 < 8:
                        soft(h + 1)
                    p_v(h)
                    if fill is not None:
                        proj_q_head(fill, h)
                for hf in range(2):
                    for h in range(8):
                        P.mm(pb[hf], oT[sl][:, h, :], wo[:, h, hf * 512:(hf + 1) * 512], h == 0, h == 7,
                             [t_o[sl], t_wos], [pbt[hf]])
                ln_epilogue(LW, [pb[0], pb[1]], [pbt[0], pbt[1]], tile_src(x_in, ctx_in, t), g1x if lat else g1c, lng, lnb,
                            [t_rows], xs1[t * 128:(t + 1) * 128, :])

            for t0 in (0, 1):
                proj(t0)
                for h in range(8):
                    proj_q_head(t0, h)
            FILL = not (dbg or {}).get("nofill", False)

            def qall(t):
                for h in range(8):
                    proj_q_head(t, h)
            attn(0)
            proj(2)
            if not FILL:
                qall(2)
            attn(1, fill=2 if FILL else None)
            for t in range(2, NT):
                if t + 1 < NT:
                    proj(t + 1)
                    if not FILL:
                        qall(t + 1)
                attn(t, fill=(t + 1) if (FILL and t + 1 < NT) else None)
            P.barrier()
            AR.top = base_mark

        def ffn_phase(src, dst_fn, tiles_all, tb_tiles, experts, li, ln_i, moe, s_of_tile):
            hTb = AR.alloc([8, 1024], BF16)
            yacc = AR.alloc([8, 1024], F32)
            aT = [AR.alloc([4, 1024], BF16) for _ in range(2)]
            wgu = [AR.alloc([8, 1024], BF16) for _ in range(2)]
            wdn = [AR.alloc([4, 1024], BF16) for _ in range(2)]
            sgt = [AR.alloc([512], F32) for _ in range(2)]
            g2x, g2c, lng, lnb = (AR.alloc([D], F32) for _ in range(4))
            t_rows = Tok()
            t_hT, t_y = Tok(), [Tok() for _ in range(8)]
            t_aT = [Tok(), Tok()]
            t_wg = [Tok(), Tok()]
            t_wd = [Tok(), Tok()]
            t_sg = [Tok(), Tok()]
            TW = TPWork(2)
            LW = LNWork(2)
            load_rows(g2x, modrow[0, li, 5 * D:6 * D], t_rows)
            load_rows(g2c, modrow[1, li, 5 * D:6 * D], t_rows)
            load_rows(lng, ln_g[li, ln_i, :], t_rows)
            load_rows(lnb, ln_b[li, ln_i, :], t_rows)
            if moe:
                hTf = [AR.alloc([8, 128], F32) for _ in range(2)]
                t_hf = [Tok(), Tok()]
                rsb = AR.alloc([8, NE], F32)
                gates = AR.alloc([8, NE], F32)
                rw = [AR.alloc([48], F32) for _ in range(2)]
                t_rsb, t_g, t_rw = Tok(), [Tok() for _ in range(8)], [Tok(), Tok()]
                P.ld(rsb, m_router.rearrange("(c p) e -> p c e", p=128), [], [t_rsb])
            step = 0
            gslot = 0
            for tb in tb_tiles:
                ntile = len(tb)
                ntok = ntile * 128
                halves = [(o, min(512, ntok - o)) for o in range(0, ntok, 512)]
                for ti, t in enumerate(tb):
                    s = s_of_tile(t)
                    if moe:
                        u = ti % 2
                        mod_transpose(TW, src[t * 128:(t + 1) * 128, :], dst_tok_of(src), s, li, 3, 4,
                                      lambda c, u=u: hTf[u][:, c, :], t_hf[u], banks=(0, 1))
                        P.cp(hTb[:, :, ti * 128:(ti + 1) * 128], hTf[u], [t_hf[u]], [t_hT], eng="pool")
                        mpro = (dbg or {}).get("moe_pro", 9)
                        if mpro < 1:
                            continue
                        for c in range(8):
                            P.mm(pb[6][:, 0:NE], hTf[u][:, c, :], rsb[:, c, :], c == 0, c == 7, [t_hf[u], t_rsb], [pbt[6]])
                        r = rw[u]
                        tr_ = t_rw[u]
                        if mpro < 2:
                            P.cp(r[:, 0:8], pb[6][:, 0:NE], [pbt[6]], [tr_])
                            continue
                        P.cp(r[:, 0:8], pb[6][:, 0:NE], [pbt[6]], [tr_])
                        P.op("dve", lambda e, r=r: e.max(out=r[:, 8:16], in_=r[:, 0:8]), [], [tr_])
                        P.ts(r[:, 16:24], r[:, 0:8], r[:, 8:9], None, ALU.is_equal, None, [], [tr_])
                        P.ts(r[:, 24:32], r[:, 0:8], r[:, 9:10], None, ALU.is_equal, None, [], [tr_])
                        P.ts(r[:, 35:36], r[:, 8:9], -1.0, None, ALU.mult, None, [], [tr_])
                        P.act(r[:, 32:33], r[:, 9:10], AF.Exp, [], [tr_], bias=r[:, 35:36], scale=1.0)
                        P.ts(r[:, 33:34], r[:, 32:33], 1.0, None, ALU.add, None, [], [tr_])
                        P.op("dve", lambda e, r=r: e.reciprocal(out=r[:, 33:34], in_=r[:, 33:34]), [], [tr_])
                        P.tt(r[:, 34:35], r[:, 32:33], r[:, 33:34], ALU.mult, [], [tr_])
                        P.ts(r[:, 16:24], r[:, 16:24], r[:, 33:34], None, ALU.mult, None, [], [tr_])
                        P.stt(gates[:, ti, :], r[:, 24:32], r[:, 34:35], r[:, 16:24], ALU.mult, ALU.add, [], [tr_, t_g[ti]])
                    else:
                        mod_transpose(TW, src[t * 128:(t + 1) * 128, :], dst_tok_of(src), s, li, 3, 4,
                                      lambda c, ti=ti: hTb[:, c, ti * 128:(ti + 1) * 128], t_hT, banks=(0, 1))
                units = [(ei, e, g) for ei, e in enumerate(experts) for g in range(NG)]
                mstage = (dbg or {}).get("moe_stage", 2) if li == 1 else 2
                if mstage < 1:
                    continue
                def emit_dn_group(ei, e, g, sl, ti, hf):
                    nonlocal gslot
                    bk = 6 + (gslot % 2)
                    gslot += 1
                    for j in range(4):
                        P.mm(pb[bk], aT[sl][:, j, ti * 128:(ti + 1) * 128], wdn[sl][:, j, hf * 512:(hf + 1) * 512],
                             j == 0, j == 3, [t_aT[sl], t_wd[sl]], [pbt[bk]])
                    ya = yacc[:, ti, hf * 512:(hf + 1) * 512]
                    first = (ei == 0 and g == 0)
                    if moe:
                        gsc = gates[:, ti, ei:ei + 1]
                        if first:
                            P.ts(ya, pb[bk], gsc, None, ALU.mult, None, [pbt[bk], t_g[ti]], [t_y[ti]])
                        else:
                            P.stt(ya, pb[bk], gsc, ya, ALU.mult, ALU.add, [pbt[bk], t_g[ti]], [t_y[ti]])
                    else:
                        if first:
                            P.cp(ya, pb[bk], [pbt[bk]], [t_y[ti]])
                        else:
                            P.tt(ya, pb[bk], ya, ALU.add, [pbt[bk]], [t_y[ti]])

                pend = []
                for (ei, e, g) in units:
                    sl = step % 2
                    step += 1
                    P.ld(wgu[sl], wgu_s[e, g].rearrange("(c p) n -> p c n", p=128), [t_wgu[e][g]], [t_wg[sl]])
                    P.ld(wdn[sl], wdn_s[e, g * 512:(g + 1) * 512, :].rearrange("(j p) n -> p j n", p=128), [t_wdn[e]], [t_wd[sl]])
                    per = -(-len(pend) // (4 * len(halves)))
                    for j in range(4):
                        for hi, (ho, hw) in enumerate(halves):
                            pr = (j * len(halves) + hi) % 2
                            bg, bu = 2 + 2 * pr, 3 + 2 * pr
                            for c in range(8):
                                P.mm(pb[bg][:, 0:hw], wgu[sl][:, c, j * 128:(j + 1) * 128], hTb[:, c, ho:ho + hw], c == 0, c == 7,
                                     [t_wg[sl], t_hT], [pbt[bg]])
                            for c in range(8):
                                P.mm(pb[bu][:, 0:hw], wgu[sl][:, c, 512 + j * 128:512 + (j + 1) * 128], hTb[:, c, ho:ho + hw], c == 0, c == 7,
                                     [t_wg[sl], t_hT], [pbt[bu]])
                            P.act(sgt[pr][:, 0:hw], pb[bg][:, 0:hw], AF.Silu, [pbt[bg]], [t_sg[pr]])
                            P.tt(aT[sl][:, j, ho:ho + hw], sgt[pr][:, 0:hw], pb[bu][:, 0:hw], ALU.mult, [t_sg[pr], pbt[bu]], [t_aT[sl]])
                            for _ in range(per):
                                if pend:
                                    emit_dn_group(*pend.pop(0))
                    while pend:
                        emit_dn_group(*pend.pop(0))
                    pend = [(ei, e, g, sl, ti, hf) for ti in range(ntile) for hf in range(2)]
                while pend:
                    emit_dn_group(*pend.pop(0))
                if mstage < 2:
                    continue
                for ti, t in enumerate(tb):
                    lat = s_of_tile(t) == 0
                    ln_epilogue(LW, [yacc[:, ti, 0:512], yacc[:, ti, 512:1024]], [t_y[ti]], src[t * 128:(t + 1) * 128, :],
                                g2x if lat else g2c, lng, lnb, [t_rows], dst_fn(t), xres_toks=dst_tok_of(src))

        if dbg is None or dbg.get("ffn", True):
            tbs = [list(range(0, 7)), list(range(7, 14)), list(range(14, 21)), list(range(21, 28)), list(range(28, 34))]
            ffn_phase(xs1, lambda t: xs2[t * 128:(t + 1) * 128, :], list(range(NT)), tbs, [0], 0, 1, False,
                      lambda t: 1 if t < 2 else 0)
            P.barrier()
            AR.top = base_mark

        if dbg is None or dbg.get("lru", True):
            win = AR.alloc([8, 2048], BF16)
            hTd = [AR.alloc([8, 512], BF16) for _ in range(2)]
            gst = [AR.alloc([512], BF16) for _ in range(2)]
            xst = [AR.alloc([512], F32) for _ in range(2)]
            t_win2, t_hd, t_gs, t_xs = Tok(), [Tok(), Tok()], [Tok(), Tok()], [Tok(), Tok()]
            t_xb = [Tok() for _ in range(8)]
            t_gate = [Tok() for _ in range(8)]
            t_ys = [Tok() for _ in range(8)]
            TW = TPWork(2)
            P.ld(win, win_s.rearrange("(c p) n -> p c n", p=128), [t_win], [t_win2])
            blocks = [[0, 1]] + [list(range(2 + 4 * k, 6 + 4 * k)) for k in range(8)]
            kk = 0
            for bi, tb in enumerate(blocks):
                sl = bi % 2
                ntok = len(tb) * 128
                tok0 = tb[0] * 128
                for ti, t in enumerate(tb):
                    mod_transpose(TW, xs2[t * 128:(t + 1) * 128, :], dst_tok_of(xs2), 1 if t < 2 else 0, 1, 0, 1,
                                  lambda c, ti=ti: hTd[sl][:, c, ti * 128:(ti + 1) * 128], t_hd[sl])
                for n in range(8):
                    u = kk % 2
                    kk += 1
                    if bi > 0:
                        for c in range(8):
                            P.mm(pb[2 + u][:, 0:ntok], win[:, c, n * 128:(n + 1) * 128], hTd[sl][:, c, 0:ntok], c == 0, c == 7,
                                 [t_win2, t_hd[sl]], [pbt[2 + u]])
                        P.act(gst[u][:, 0:ntok], pb[2 + u][:, 0:ntok], AF.Gelu, [pbt[2 + u]], [t_gs[u]])
                        P.ld(gate_s[n][:, tok0 - LC:tok0 - LC + ntok], gst[u][:, 0:ntok], [t_gs[u]], [t_gate[n]])
                    for c in range(8):
                        P.mm(pb[4 + u][:, 0:ntok], win[:, c, D + n * 128:D + (n + 1) * 128], hTd[sl][:, c, 0:ntok], c == 0, c == 7,
                             [t_win2, t_hd[sl]], [pbt[4 + u]])
                    P.cp(xst[u][:, 0:ntok], pb[4 + u][:, 0:ntok], [pbt[4 + u]], [t_xs[u]])
                    P.ld(xb_s[n][:, tok0:tok0 + ntok], xst[u][:, 0:ntok], [t_xs[u]], [t_xb[n]])
            P.barrier()
            AR.top = base_mark

            lru_stop = (dbg or {}).get('lru_stop', 3)
            if lru_stop >= 2:
                XB = AR.alloc([NQ + 3], F32)
                U = AR.alloc([NQ], F32)
                Ub = AR.alloc([NQ + 1], BF16)
                Rr = [AR.alloc([NQ], F32) for _ in range(2)]
                Iis = [AR.alloc([NQ], F32) for _ in range(2)]
                Aas = [AR.alloc([NQ], F32) for _ in range(2)]
                Gt = AR.alloc([L], BF16)
                Yt = AR.alloc([L], BF16)
                wrb = AR.alloc([16, 128], BF16)
                wib = AR.alloc([16, 128], BF16)
                t_XB, t_U, t_Ub, t_R, t_Is, t_As, t_G, t_Y, t_wrb = Tok(), Tok(), Tok(), [Tok(), Tok()], [Tok(), Tok()], [Tok(), Tok()], Tok(), Tok(), Tok()
                P.op("dve", lambda e: e.memset(XB, 0.0), [], [t_XB])
                P.ld(wrb, wr_s.rearrange("(a p) n -> p a n", p=128), [t_wr], [t_wrb])
                P.ld(wib, wi_s.rearrange("(a p) n -> p a n", p=128), [t_wi], [t_wrb])
                cblocks = [(o, min(512, NQ - o)) for o in range(0, NQ, 512)]
                kk = 0
                for n in range(8):
                    P.ld(XB[:, 2:258], xb_s[n][:, 0:LC], [t_xb[n]], [t_XB])
                    P.ld(XB[:, 261:261 + L], xb_s[n][:, LC:T], [t_xb[n]], [t_XB])
                    P.ld(Gt, gate_s[n], [t_gate[n]], [t_G])
                    P.ts(U, XB[:, 0:NQ], lruT[:, n:n + 1], lruT[:, 32 + n:33 + n], ALU.mult, ALU.add, [t_XB, t_lruT], [t_U])
                    for j in range(1, 4):
                        P.stt(U, XB[:, j:j + NQ], lruT[:, j * 8 + n:j * 8 + n + 1], U, ALU.mult, ALU.add, [t_XB, t_lruT], [t_U])
                    P.cp(Ub[:, 0:NQ], U, [t_U], [t_Ub], eng="act")
                    for d in range(2):
                        R = Rr[d]
                        tR = t_R[d]
                        Ii, Aa, t_I, t_A = Iis[d], Aas[d], t_Is[d], t_As[d]
                        for (o, w) in cblocks:
                            u = kk % 2
                            kk += 1
                            P.mm(pb[2 + u][:, 0:w], wrb[:, d * 8 + n, :], Ub[:, o:o + w], True, True, [t_wrb, t_Ub], [pbt[2 + u]])
                            P.act(R[:, o:o + w], pb[2 + u][:, 0:w], AF.Sigmoid, [pbt[2 + u], t_lruT], [tR],
                                  bias=lruT[:, 56 + d * 8 + n:57 + d * 8 + n], scale=1.0)
                            P.mm(pb[4 + u][:, 0:w], wib[:, d * 8 + n, :], Ub[:, o:o + w], True, True, [t_wrb, t_Ub], [pbt[4 + u]])
                            P.act(Ii[:, o:o + w], pb[4 + u][:, 0:w], AF.Sigmoid, [pbt[4 + u], t_lruT], [t_I],
                                  bias=lruT[:, 72 + d * 8 + n:73 + d * 8 + n], scale=1.0)
                        P.act(Aa, R, AF.Exp, [tR, t_lruT], [t_A], scale=lruT[:, 88 + d * 8 + n:89 + d * 8 + n])
                        P.act(R, R, AF.Exp, [t_lruT], [tR], scale=lruT[:, 104 + d * 8 + n:105 + d * 8 + n])
                        P.ts(R, R, 1.0, None, ALU.min, None, [], [tR])
                        P.act(R, R, AF.Sqrt, [], [tR], bias=1.0, scale=-1.0)
                        P.tt(Ii, Ii, U, ALU.mult, [t_U], [t_I], eng="pool")
                        P.tt(Ii, Ii, R, ALU.mult, [tR], [t_I])
                        if d == 0:
                            P.op("dve", lambda e, R=R, Aa=Aa, Ii=Ii: e.tensor_tensor_scan(out=R[:, 0:LC], data0=Aa[:, 0:LC], data1=Ii[:, 0:LC],
                                                                              initial=0.0, op0=ALU.mult, op1=ALU.add), [t_A, t_I], [tR])
                            P.op("dve", lambda e, R=R, Aa=Aa, Ii=Ii: e.tensor_tensor_scan(out=R[:, 259:NQ], data0=Aa[:, 259:NQ], data1=Ii[:, 259:NQ],
                                                                              initial=R[:, LC - 1:LC], op0=ALU.mult, op1=ALU.add), [t_A, t_I], [tR])
                        else:
                            P.op("dve", lambda e, R=R, Aa=Aa, Ii=Ii: e.tensor_tensor_scan(out=rev(R[:, 0:LC]), data0=rev(Aa[:, 0:LC]), data1=rev(Ii[:, 0:LC]),
                                                                              initial=0.0, op0=ALU.mult, op1=ALU.add), [t_A, t_I], [tR])
                            P.op("dve", lambda e, R=R, Aa=Aa, Ii=Ii: e.tensor_tensor_scan(out=rev(R[:, 259:NQ]), data0=rev(Aa[:, 259:NQ]), data1=rev(Ii[:, 259:NQ]),
                                                                              initial=R[:, 0:1], op0=ALU.mult, op1=ALU.add), [t_A, t_I], [tR])
                    P.tt(Iis[0][:, 0:L], Rr[0][:, 259:NQ], Rr[1][:, 259:NQ], ALU.add, [t_R[0], t_R[1]], [t_Is[0]], eng="pool")
                    P.tt(Yt, Iis[0][:, 0:L], Gt, ALU.mult, [t_G], [t_Y, t_Is[0]], eng="pool")
                    P.ld(y_s[n], Yt, [t_Y], [t_ys[n]])
                P.barrier()
                AR.top = base_mark

            if lru_stop >= 3:
                wout = AR.alloc([8, 1024], BF16)
                Yb = [AR.alloc([8, 512], BF16) for _ in range(2)]
                g1x, lng, lnb = (AR.alloc([D], F32) for _ in range(3))
                t_wo2, t_Yb, t_rows = Tok(), [Tok(), Tok()], Tok()
                LW = LNWork(2)
                P.ld(wout, wout_s.rearrange("(c p) n -> p c n", p=128), [t_wout], [t_wo2])
                load_rows(g1x, modrow[0, 1, 2 * D:3 * D], t_rows)
                load_rows(lng, ln_g[1, 0, :], t_rows)
                load_rows(lnb, ln_b[1, 0, :], t_rows)
                ysv = y_s.rearrange("n p t -> p n t")
                kk = 0
                for b in range(8):
                    sl = b % 2
                    for n in range(8):
                        P.ld(Yb[sl][:, n, :], y_s[n][:, b * 512:(b + 1) * 512], [t_ys[n]], [t_Yb[sl]])
                    for ti in range(4):
                        t = b * 4 + ti
                        u = kk % 2
                        kk += 1
                        for hf in range(2):
                            bk = 2 + 2 * u + hf
                            for n in range(8):
                                P.mm(pb[bk], Yb[sl][:, n, ti * 128:(ti + 1) * 128], wout[:, n, hf * 512:(hf + 1) * 512], n == 0, n == 7,
                                     [t_Yb[sl], t_wo2], [pbt[bk]])
                        ln_epilogue(LW, [pb[2 + 2 * u], pb[3 + 2 * u]], [pbt[2 + 2 * u], pbt[3 + 2 * u]],
                                    xs2[LC + t * 128:LC + (t + 1) * 128, :], g1x, lng, lnb, [t_rows], xs3[t * 128:(t + 1) * 128, :], xres_toks=dst_tok_of(xs2))
                P.barrier()
                AR.top = base_mark

        if dbg is None or (dbg.get("moe", True) and dbg.get("moe_run", True)):
            tbs = [list(range(8 * k, 8 * k + 8)) for k in range(4)]
            moe_src = xs3
            moe_exp = list(range(1, 1 + NE))
            if dbg and dbg.get("moe_src_x"):
                moe_src = x_in
            if dbg and "moe_ntb" in dbg:
                tbs = tbs[:dbg["moe_ntb"]]
            if dbg and "moe_exp" in dbg:
                moe_exp = dbg["moe_exp"]
            ffn_phase(moe_src, lambda t: out[t * 128:(t + 1) * 128, :], list(range(32)), tbs, moe_exp, 1, 1,
                      not (dbg or {}).get("moe_dense", False), lambda t: 0)

        if dbg and dbg.get("wait_casts"):
            P.wait("sp", [tk for lst in t_wgu for tk in lst] + t_wdn)
        P.wait("sp", [tk for tk in dst_toks.values()] + [t_modrow])
        P.emit(st)
    return nc


def _consts():
    ident = np.eye(128, dtype=np.float32)
    t = np.arange(L)
    row = (t // 64).astype(np.float32)
    col = (t % 64).astype(np.float32)
    freqs = (10000.0 ** (-np.arange(0, 64, 2, dtype=np.float32) / 64)).astype(np.float32)
    ra = row[None, :] * freqs[:, None]
    ca = col[None, :] * freqs[:, None]
    cosT = np.concatenate([np.cos(ra), np.cos(ra), np.cos(ca), np.cos(ca)], axis=0).astype(np.float32)
    sinT = np.concatenate([-np.sin(ra), np.sin(ra), -np.sin(ca), np.sin(ca)], axis=0).astype(np.float32)
    qi = np.arange(128)[:, None]
    j = np.arange(128)[None, :]
    lo = (j >= qi).astype(np.float32)
    mid = np.ones((128, 128), np.float32)
    hi = (j <= qi).astype(np.float32)
    z = np.zeros((128, 128), np.float32)
    mask = np.concatenate([np.concatenate([z, mid, hi], 1), np.concatenate([lo, mid, hi], 1), np.concatenate([lo, mid, z], 1)], 1)
    return ident, cosT, sinT, np.ascontiguousarray(mask)


def make_in_maps(inputs, ncores=8):
    g = lambda k: np.ascontiguousarray(np.asarray(inputs[k], dtype=np.float32))
    ident, cosT, sinT, mask = _consts()
    shared = {
        "w_mod": g("w_mod"), "b_mod": g("b_mod"), "ln_g": g("ln_g"), "ln_b": g("ln_b"),
        "attn_w_qkv": g("attn_w_qkv")[0], "attn_w_o": g("attn_w_o")[0], "attn_sink": g("attn_sink").reshape(1, 8),
        "ffn_w_gu": g("ffn_w_gu")[0], "ffn_w_dn": g("ffn_w_dn")[0],
        "lru_w_in": g("lru_w_in")[0], "lru_conv_w": g("lru_conv_w")[0], "lru_conv_b": g("lru_conv_b").reshape(1, D),
        "lru_lambda": g("lru_lambda")[0], "lru_w_r": g("lru_w_r").reshape(2048, 128), "lru_b_r": g("lru_b_r")[0],
        "lru_w_i": g("lru_w_i").reshape(2048, 128), "lru_b_i": g("lru_b_i")[0], "lru_w_out": g("lru_w_out")[0],
        "moe_router": g("moe_router")[0], "moe_w_gu": g("moe_w_gu")[0], "moe_w_dn": g("moe_w_dn")[0],
        "c_ident": ident, "c_cos": cosT, "c_sin": sinT, "c_mask": mask,
    }
    x, c, ctx, c_ctx = g("x"), g("c"), g("ctx"), g("c_ctx")
    maps = []
    for b in range(ncores):
        m = dict(shared)
        m["x"] = x[b]
        m["ctx"] = ctx[b]
        m["cvec"] = np.ascontiguousarray(np.stack([c[b], c_ctx], 0))
        maps.append(m)
    return maps


_NC = None


def kernel(**inputs):
    global _NC
    if _NC is None:
        _NC = build()
    maps = make_in_maps(inputs)
    res = run_bass_kernel_spmd(_NC, maps, core_ids=list(range(8)))
    return np.stack([np.asarray(r["out"], dtype=np.float32) for r in res.results], 0)
```
